# Optimizing a Trainium2 kernel written in Bass

```python
import math
import jax
import jax.numpy as jnp
from jax import lax
import numpy as np

D_MODEL = 1024
BATCH = 4
SEQ = 8192
DEPTH = 2

CHUNK = 64
Q_BLOCK = 128
MEM_LEN = 256
MEM_HEADS = 4
MEM_DIM = D_MODEL // MEM_HEADS

FOX_DIM = 64
FOX_W = D_MODEL // 4
FOX_HEADS = FOX_W // FOX_DIM
DIFF_QK_DIM = 32
DIFF_V_DIM = 2 * DIFF_QK_DIM
DIFF_W = D_MODEL // 4
DIFF_HEADS = DIFF_W // DIFF_V_DIM
HGRN_DIM = 128
HGRN_W = D_MODEL - FOX_W - DIFF_W
HGRN_HEADS = HGRN_W // HGRN_DIM

MIX_W = FOX_W + DIFF_W + HGRN_W
IN_SIZES = (FOX_W, FOX_W, FOX_W, FOX_HEADS, DIFF_W, DIFF_W, DIFF_W, HGRN_W, HGRN_W, HGRN_W, HGRN_W)
IN_W = sum(IN_SIZES)

N_EXPERTS = 16
N_GROUPS = 4
EXPERTS_PER_GROUP = N_EXPERTS // N_GROUPS
TOP_K = 2
EXPERT_FF = D_MODEL // 2

ALPHA = (2 * DEPTH) ** 0.25
BETA = (8 * DEPTH) ** -0.25
LN_EPS = 1e-5

kernel_name = 'hybrid_fox_diff_hgrn2_moe_encoder'


def _layer_norm(x, g, b):
    xf = x.astype(jnp.float32)
    mu = jnp.mean(xf, axis=-1, keepdims=True)
    var = jnp.mean(jnp.square(xf - mu), axis=-1, keepdims=True)
    y = (xf - mu) * lax.rsqrt(var + LN_EPS) * g.astype(jnp.float32) + b.astype(jnp.float32)
    return y.astype(x.dtype)


def _rms_norm(x, g):
    xf = x.astype(jnp.float32)
    y = xf * lax.rsqrt(jnp.mean(xf * xf, axis=-1, keepdims=True) + LN_EPS) * g.astype(jnp.float32)
    return y.astype(x.dtype)


def _split_heads(t, n_heads):
    b, s, _ = t.shape
    return t.reshape(b, s, n_heads, -1).transpose(0, 2, 1, 3)


def _merge_heads(t):
    b, h, s, d = t.shape
    return t.transpose(0, 2, 1, 3).reshape(b, s, h * d)


def _to_blocks(t):
    b, h, s = t.shape[:3]
    t = t.reshape((b, h, s // Q_BLOCK, Q_BLOCK) + t.shape[3:])
    return jnp.moveaxis(t, 2, 0)


def _from_blocks(t):
    nb, b, h, q, d = t.shape
    return t.transpose(1, 2, 0, 3, 4).reshape(b, h, nb * q, d)


def _split_columns(proj):
    parts, start = [], 0
    for width in IN_SIZES:
        parts.append(proj[..., start:start + width])
        start += width
    return parts


def _forgetting_attention(q, k, v, log_f):
    s = q.shape[2]
    cum = jnp.cumsum(log_f, axis=-1)
    key_pos = jnp.arange(s)
    query_pos = key_pos.reshape(s // Q_BLOCK, Q_BLOCK)
    scale = FOX_DIM ** -0.5

    def block(args):
        q_blk, c_blk, q_pos = args
        logits = jnp.einsum('bhqd,bhkd->bhqk', q_blk, k).astype(jnp.float32) * scale
        logits = logits + c_blk[..., :, None] - cum[..., None, :]
        logits = jnp.where(key_pos[None, :] <= q_pos[:, None], logits, -jnp.inf)
        p = jax.nn.softmax(logits, axis=-1).astype(v.dtype)
        return jnp.einsum('bhqk,bhkd->bhqd', p, v)

    out = lax.map(block, (_to_blocks(q), _to_blocks(cum), query_pos))
    return _from_blocks(out)


def _differential_attention(q1, q2, k1, k2, v, lam):
    s = q1.shape[2]
    key_chunk = jnp.arange(s) // CHUNK
    query_chunk = key_chunk.reshape(s // Q_BLOCK, Q_BLOCK)
    scale = DIFF_QK_DIM ** -0.5

    def block(args):
        q1b, q2b, qc = args
        visible = key_chunk[None, :] <= qc[:, None]

        def attn_map(qb, kk):
            logits = jnp.einsum('bhqd,bhkd->bhqk', qb, kk).astype(jnp.float32) * scale
            return jax.nn.softmax(jnp.where(visible, logits, -jnp.inf), axis=-1)

        p = attn_map(q1b, k1) - lam * attn_map(q2b, k2)
        return jnp.einsum('bhqk,bhkd->bhqd', p.astype(v.dtype), v)

    out = lax.map(block, (_to_blocks(q1), _to_blocks(q2), query_chunk))
    return _from_blocks(out)


def _hgrn2_recurrence(q, k, log_f, v):
    b, h, s, dk = q.shape
    dv = v.shape[-1]
    n = s // CHUNK

    def chunks(t):
        t = t.astype(jnp.float32)
        return t.reshape(b, h, n, CHUNK, t.shape[-1]).transpose(2, 0, 1, 3, 4)

    pos = jnp.arange(CHUNK)
    causal = (pos[:, None] >= pos[None, :])[None, None, :, :, None]

    def step(state, inp):
        qc, kc, gc, vc = inp
        bc = jnp.cumsum(gc, axis=2)
        rel = jnp.where(causal, bc[:, :, :, None, :] - bc[:, :, None, :, :], -jnp.inf)
        scores = jnp.einsum('bhtd,bhsd,bhtsd->bhts', qc, kc, jnp.exp(rel))
        o = (jnp.einsum('bhts,bhsv->bhtv', scores, vc)
             + jnp.einsum('bhtd,bhdv->bhtv', qc * jnp.exp(bc), state))
        b_last = bc[:, :, -1, :]
        k_dec = kc * jnp.exp(b_last[:, :, None, :] - bc)
        state = jnp.exp(b_last)[..., None] * state + jnp.einsum('bhsd,bhsv->bhdv', k_dec, vc)
        return state, o

    state0 = jnp.zeros((b, h, dk, dv), jnp.float32)
    _, o = lax.scan(step, state0, (chunks(q), chunks(k), chunks(log_f), chunks(v)))
    return o.transpose(1, 2, 0, 3, 4).reshape(b, h, s, dv)


def _hybrid_mixer(x, w_in, fox_fb, lam_q1, lam_k1, lam_q2, lam_k2, lam_init,
                  diff_norm_g, lower_bound, hgrn_norm_g, w_out):
    proj = x @ w_in
    fq, fk, fv, ff, dq, dk, dv, hq, hf, hi, hg = _split_columns(proj)

    log_f = jax.nn.log_sigmoid(ff.astype(jnp.float32) + fox_fb.astype(jnp.float32))
    y_fox = _forgetting_attention(_split_heads(fq, FOX_HEADS), _split_heads(fk, FOX_HEADS),
                                  _split_heads(fv, FOX_HEADS), log_f.transpose(0, 2, 1))

    dq_h = _split_heads(dq, DIFF_HEADS)
    dk_h = _split_heads(dk, DIFF_HEADS)
    lam = (jnp.exp(jnp.sum(lam_q1.astype(jnp.float32) * lam_k1.astype(jnp.float32)))
           - jnp.exp(jnp.sum(lam_q2.astype(jnp.float32) * lam_k2.astype(jnp.float32)))
           + lam_init)
    y_diff = _differential_attention(dq_h[..., :DIFF_QK_DIM], dq_h[..., DIFF_QK_DIM:],
                                     dk_h[..., :DIFF_QK_DIM], dk_h[..., DIFF_QK_DIM:],
                                     _split_heads(dv, DIFF_HEADS), lam)
    y_diff = _rms_norm(y_diff, diff_norm_g) * (1.0 - lam_init)

    f_logit = hf.astype(jnp.float32)
    lb = lower_bound.astype(jnp.float32)
    log_f_h = jnp.logaddexp(jnp.log(lb), jnp.log1p(-lb) + jax.nn.log_sigmoid(f_logit))
    k_h = (1.0 - lb) * jax.nn.sigmoid(-f_logit)
    o = _hgrn2_recurrence(_split_heads(jax.nn.silu(hq), HGRN_HEADS), _split_heads(k_h, HGRN_HEADS),
                          _split_heads(log_f_h, HGRN_HEADS), _split_heads(hi, HGRN_HEADS))
    y_hgrn = _rms_norm(o, hgrn_norm_g) * jax.nn.silu(_split_heads(hg, HGRN_HEADS).astype(jnp.float32))

    y = jnp.concatenate([_merge_heads(y_fox).astype(x.dtype), _merge_heads(y_diff).astype(x.dtype),
                         _merge_heads(y_hgrn).astype(x.dtype)], axis=-1)
    return y @ w_out


def _memory_attention(x, mem, w_q, w_k, w_v, w_o):
    q = _split_heads(x @ w_q, MEM_HEADS)
    k = _split_heads(mem @ w_k, MEM_HEADS)
    v = _split_heads(mem @ w_v, MEM_HEADS)
    logits = jnp.einsum('bhqd,bhkd->bhqk', q, k).astype(jnp.float32) * MEM_DIM ** -0.5
    p = jax.nn.softmax(logits, axis=-1).astype(v.dtype)
    return _merge_heads(jnp.einsum('bhqk,bhkd->bhqd', p, v)) @ w_o


def _grouped_moe(x, router_w, router_b, w1, w3, w2):
    b, s, d = x.shape
    xt = x.reshape(b * s, d)
    n = xt.shape[0]
    scores = jax.nn.sigmoid((xt @ router_w).astype(jnp.float32))
    biased = scores + router_b.astype(jnp.float32)
    grouped = biased.reshape(n, N_GROUPS, EXPERTS_PER_GROUP)
    group_score = jnp.sum(lax.top_k(grouped, TOP_K)[0], axis=-1)
    group_sel = jnp.argmax(group_score, axis=-1)
    in_group = jnp.arange(N_GROUPS)[None, :] == group_sel[:, None]
    masked = jnp.where(in_group[:, :, None], grouped, -jnp.inf).reshape(n, N_EXPERTS)
    _, expert_idx = lax.top_k(masked, TOP_K)
    sel = jnp.take_along_axis(scores, expert_idx, axis=-1)
    gates = sel / jnp.sum(sel, axis=-1, keepdims=True)
    gate_dense = jnp.sum(jax.nn.one_hot(expert_idx, N_EXPERTS, dtype=jnp.float32) * gates[..., None], axis=1)
    out = jnp.zeros_like(xt)
    for e in range(N_EXPERTS):
        hid = jax.nn.silu(xt @ w1[e]) * (xt @ w3[e])
        out = out + gate_dense[:, e, None].astype(xt.dtype) * (hid @ w2[e])
    return out.reshape(b, s, d)


def setup_inputs(seed: int = 0) -> dict:
    key = jax.random.key(seed)
    ks = jax.random.split(key, 26)
    f32 = jnp.float32

    def nrm(k, shape, scale):
        return jax.random.normal(k, shape, f32) * scale

    d = D_MODEL
    return {
        'x': nrm(ks[0], (BATCH, SEQ, d), 1.0),
        'mem': nrm(ks[1], (BATCH, MEM_LEN, d), 1.0),
        'ln_in_g': 1.0 + nrm(ks[2], (d,), 0.02),
        'ln_in_b': nrm(ks[3], (d,), 0.02),
        'w_in': nrm(ks[4], (DEPTH, d, IN_W), d ** -0.5),
        'fox_fb': jax.random.uniform(ks[5], (DEPTH, FOX_HEADS), f32, 1.0, 3.0),
        'lam_q1': nrm(ks[6], (DEPTH, DIFF_QK_DIM), 0.1),
        'lam_k1': nrm(ks[7], (DEPTH, DIFF_QK_DIM), 0.1),
        'lam_q2': nrm(ks[8], (DEPTH, DIFF_QK_DIM), 0.1),
        'lam_k2': nrm(ks[9], (DEPTH, DIFF_QK_DIM), 0.1),
        'diff_norm_g': 1.0 + nrm(ks[10], (DEPTH, DIFF_V_DIM), 0.02),
        'hgrn_lb': nrm(ks[11], (DEPTH, HGRN_W), 0.1),
        'hgrn_norm_g': 1.0 + nrm(ks[12], (DEPTH, HGRN_DIM), 0.02),
        'w_out': nrm(ks[13], (DEPTH, MIX_W, d), BETA * MIX_W ** -0.5),
        'mem_wq': nrm(ks[14], (DEPTH, d, d), d ** -0.5),
        'mem_wk': nrm(ks[15], (DEPTH, d, d), d ** -0.5),
        'mem_wv': nrm(ks[16], (DEPTH, d, d), d ** -0.5),
        'mem_wo': nrm(ks[17], (DEPTH, d, d), BETA * d ** -0.5),
        'router_w': nrm(ks[18], (d, N_EXPERTS), d ** -0.5),
        'router_b': nrm(ks[19], (N_EXPERTS,), 0.01),
        'w1': nrm(ks[20], (DEPTH, N_EXPERTS, d, EXPERT_FF), d ** -0.5),
        'w3': nrm(ks[21], (DEPTH, N_EXPERTS, d, EXPERT_FF), d ** -0.5),
        'w2': nrm(ks[22], (DEPTH, N_EXPERTS, EXPERT_FF, d), BETA * EXPERT_FF ** -0.5),
        'ln_g': 1.0 + nrm(ks[23], (DEPTH, 3, d), 0.02),
        'ln_b': nrm(ks[24], (DEPTH, 3, d), 0.02),
    }


def reference(x, mem, ln_in_g, ln_in_b, w_in, fox_fb, lam_q1, lam_k1, lam_q2, lam_k2,
              diff_norm_g, hgrn_lb, hgrn_norm_g, w_out, mem_wq, mem_wk, mem_wv, mem_wo,
              router_w, router_b, w1, w3, w2, ln_g, ln_b):
    lbs = jax.nn.softmax(hgrn_lb.astype(jnp.float32), axis=0)
    lbs = jnp.cumsum(lbs, axis=0) - lbs[0]
    h = _layer_norm(x, ln_in_g, ln_in_b)
    for i in range(DEPTH):
        lam_init = 0.8 - 0.6 * math.exp(-0.3 * i)
        y = _hybrid_mixer(h, w_in[i], fox_fb[i], lam_q1[i], lam_k1[i], lam_q2[i], lam_k2[i],
                          lam_init, diff_norm_g[i], jnp.maximum(lbs[i], 0.0), hgrn_norm_g[i], w_out[i])
        h = _layer_norm(ALPHA * h + y, ln_g[i, 0], ln_b[i, 0])
        y = _memory_attention(h, mem, mem_wq[i], mem_wk[i], mem_wv[i], mem_wo[i])
        h = _layer_norm(ALPHA * h + y, ln_g[i, 1], ln_b[i, 1])
        y = _grouped_moe(h, router_w, router_b, w1[i], w3[i], w2[i])
        h = _layer_norm(ALPHA * h + y, ln_g[i, 2], ln_b[i, 2])
    return h
```

```python
import contextlib
import math
import numpy as np
import ml_dtypes
import concourse.bass as bass
import concourse.mybir as mybir
from concourse.bass_utils import run_bass_kernel_spmd

F32 = mybir.dt.float32
BF16 = mybir.dt.bfloat16
AF = mybir.ActivationFunctionType
ALU = mybir.AluOpType
AX = mybir.AxisListType

D = 1024
SEQ = 8192
BATCH = 4
DEPTH = 2
EPS = 1e-5
ALPHA = (2 * DEPTH) ** 0.25
NEXP = 16
FF = 512
MEM = 256
IN_SIZES = (256, 256, 256, 4, 256, 256, 256, 512, 512, 512, 512)

ENGS = ("pe", "dve", "act", "pool", "sp")


class Buf:
    __slots__ = ("name", "w", "r", "excl")

    def __init__(self, name="", excl=False):
        self.name = name
        self.w = None
        self.r = []
        self.excl = excl


class Tl:
    __slots__ = ("ap", "buf")

    def __init__(self, ap, buf=None, name="", excl=False):
        self.ap = ap
        self.buf = buf if buf is not None else Buf(name, excl)

    def __getitem__(self, k):
        return self.ap[k]


class Op:
    __slots__ = ("eng", "fn", "waits", "is_dma", "needs_inc", "sem", "val")


def _bufs(xs):
    out = []
    for x in xs:
        if x is None:
            continue
        out.append(x.buf if isinstance(x, Tl) else x)
    return out


class Prog:
    def __init__(self, nc, n_dma_sems=10):
        self.nc = nc
        self.ops = {e: [] for e in ENGS}
        self.n_dma_sems = n_dma_sems
        self.dma_rr = {e: 0 for e in ENGS}
        self.dma_cnt = {}
        self.dma_last = {}
        self.last = {e: None for e in ENGS}
        self.pending = {e: [] for e in ENGS}
        self.finals = []
        self.max_ops = None
        self.count = 0

    def add(self, eng, fn, reads=(), writes=(), dma=False):
        self.count += 1
        if self.max_ops is not None and self.count > self.max_ops:
            return None
        reads = _bufs(reads)
        writes = _bufs(writes)
        op = Op()
        op.eng = eng
        op.fn = fn
        op.is_dma = dma
        op.needs_inc = False
        op.sem = None
        op.val = None
        deps = []
        for b in reads:
            if b.w is not None:
                deps.append((b.w, True))
            if b.excl:
                for r in b.r:
                    if r.eng != eng:
                        deps.append((r, True))
        for b in writes:
            if b.w is not None:
                deps.append((b.w, False))
            for r in b.r:
                deps.append((r, False))
        out = []
        for d, raw in deps:
            if d is op:
                continue
            if (not d.is_dma) and (not dma) and d.eng == eng and not raw:
                continue
            out.append(d)
        out.extend(self.pending[eng])
        self.pending[eng] = []
        if dma:
            k = self.dma_rr[eng]
            self.dma_rr[eng] = (k + 1) % self.n_dma_sems
            prev = self.dma_last.get((eng, k))
            if prev is not None:
                out.append(prev)
            c = self.dma_cnt.get((eng, k), 0) + 1
            self.dma_cnt[(eng, k)] = c
            self.dma_last[(eng, k)] = op
            op.sem = (eng, k)
            op.val = 16 * c
        uniq = []
        sset = set()
        for d in out:
            if id(d) in sset:
                continue
            sset.add(id(d))
            uniq.append(d)
            if not d.is_dma:
                d.needs_inc = True
        op.waits = uniq
        self.ops[eng].append(op)
        if not dma:
            self.last[eng] = op
        for b in reads:
            b.r.append(op)
        for b in writes:
            b.w = op
            b.r = []
        return op

    def barrier(self):
        toks = [self.last[e] for e in ENGS if self.last[e] is not None]
        toks += list(self.dma_last.values())
        for e in ENGS:
            self.pending[e] = list(toks)
        for t in toks:
            if not t.is_dma:
                t.needs_inc = True

    def final(self, op):
        if op is None:
            return
        if not op.is_dma:
            op.needs_inc = True
        self.finals.append(op)

    def emit(self):
        nc = self.nc
        with contextlib.ExitStack() as es:
            esem = {e: es.enter_context(nc.semaphore("s_" + e)) for e in ENGS}
            dsem = {}
            for (e, k) in self.dma_cnt:
                dsem[(e, k)] = es.enter_context(nc.semaphore("d_%s_%d" % (e, k)))
            for e in ENGS:
                c = 0
                for op in self.ops[e]:
                    if op.is_dma:
                        op.sem = dsem[op.sem]
                    elif op.needs_inc:
                        c += 1
                        op.sem = esem[e]
                        op.val = c
            block = es.enter_context(nc.Block())
            prog = self

            def run(e, engine):
                seen = {}
                for op in prog.ops[e]:
                    need = {}
                    for d in op.waits:
                        key = id(d.sem)
                        if seen.get(key, 0) >= d.val:
                            continue
                        if key not in need or need[key][1] < d.val:
                            need[key] = (d.sem, d.val)
                    for key, (s, v) in need.items():
                        engine.wait_ge(s, v)
                        seen[key] = v
                    ins = op.fn(engine)
                    if op.is_dma:
                        ins.then_inc(op.sem, 16)
                    elif op.needs_inc:
                        ins.then_inc(op.sem, 1)

            @block.tensor
            def _(eng):
                run("pe", eng)

            @block.vector
            def _(eng):
                run("dve", eng)

            @block.scalar
            def _(eng):
                run("act", eng)

            @block.gpsimd
            def _(eng):
                run("pool", eng)

            @block.sync
            def _(eng):
                run("sp", eng)
                for d in prog.finals:
                    eng.wait_ge(d.sem, d.val)


class Arena:
    def __init__(self, nc, es, nfloats, name="arena"):
        self.t = es.enter_context(nc.sbuf_tensor(name, [128, nfloats], F32))
        self.off = 0
        self.n = nfloats

    def f32(self, shape, name=""):
        n = int(np.prod(shape[1:]))
        assert self.off + n <= self.n, ("arena overflow", name, self.off + n, self.n)
        ap = self.t[0:shape[0], self.off:self.off + n]
        self.off += n
        if len(shape) == 3:
            ap = ap.rearrange("p (a b) -> p a b", b=shape[2])
        return Tl(ap, name=name)

    def bf16(self, shape, name=""):
        n = int(np.prod(shape[1:]))
        nf = (n + 1) // 2
        assert self.off + nf <= self.n, ("arena overflow", name, self.off + nf, self.n)
        ap = self.t[0:shape[0], self.off:self.off + nf].bitcast(BF16)
        if nf * 2 != n:
            ap = ap[:, 0:n]
        self.off += nf
        if len(shape) == 3:
            ap = ap.rearrange("p (a b) -> p a b", b=shape[2])
        return Tl(ap, name=name)


def layer_norm_tile(P, z, out, gt, bt, st, mv, rs):
    def stats(e):
        zv = z.ap.rearrange("p (c f) -> p c f", f=512)
        e.bn_stats(out=st[:, 0, :], in_=zv[:, 0, :])
        return e.bn_stats(out=st[:, 1, :], in_=zv[:, 1, :])
    P.add("dve", stats, reads=[z], writes=[st])
    P.add("dve", lambda e: e.bn_aggr(out=mv.ap, in_=st.ap), reads=[st], writes=[mv])
    P.add("dve", lambda e: e.tensor_scalar_add(out=rs.ap, in0=mv[:, 1:2], scalar1=EPS), reads=[mv], writes=[rs])
    P.add("act", lambda e: e.sqrt(out=rs.ap, in_=rs.ap), reads=[rs], writes=[rs])
    P.add("dve", lambda e: e.reciprocal(out=rs.ap, in_=rs.ap), reads=[rs], writes=[rs])
    P.add("dve", lambda e: e.tensor_scalar(out=out.ap, in0=z.ap, scalar1=mv[:, 0:1], scalar2=rs[:, 0:1],
                                           op0=ALU.subtract, op1=ALU.mult), reads=[z, mv, rs], writes=[out])
    P.add("pool", lambda e: e.tensor_tensor(out=out.ap, in0=out.ap, in1=gt.ap, op=ALU.mult), reads=[out, gt], writes=[out])
    P.add("pool", lambda e: e.tensor_tensor(out=out.ap, in0=out.ap, in1=bt.ap, op=ALU.add), reads=[out, bt], writes=[out])


def transpose_tile(P, src, idt, pts, dst_bf=None, dst_f32=None):
    for hh in range(2):
        pt = pts[hh]

        def tr(e, hh=hh, pt=pt):
            ins = None
            for c in range(4):
                ins = e.transpose(out=pt[:, c, :], in_=src[:, (hh * 4 + c) * 128:(hh * 4 + c + 1) * 128], identity=idt.ap)
            return ins
        P.add("pe", tr, reads=[src, idt], writes=[pt])
        if dst_f32 is not None:
            P.add("dve", lambda e, hh=hh, pt=pt: e.tensor_copy(out=dst_f32[:, hh * 4:(hh + 1) * 4, :], in_=pt.ap),
                  reads=[pt], writes=[dst_f32])
            if dst_bf is not None:
                P.add("act", lambda e, hh=hh: e.copy(out=dst_bf[:, hh * 4:(hh + 1) * 4, :], in_=dst_f32[:, hh * 4:(hh + 1) * 4, :]),
                      reads=[dst_f32], writes=[dst_bf])
        elif dst_bf is not None:
            P.add("act", lambda e, hh=hh, pt=pt: e.copy(out=dst_bf[:, hh * 4:(hh + 1) * 4, :], in_=pt.ap),
                  reads=[pt], writes=[dst_bf])


def build_k0(T):
    nc = bass.Bass("TRN2", target_bir_lowering=False)
    NT = T // 128
    x = nc.dram_tensor("x", [T, D], F32, kind="ExternalInput").ap()
    g = nc.dram_tensor("g", [1, D], F32, kind="ExternalInput").ap()
    b = nc.dram_tensor("b", [1, D], F32, kind="ExternalInput").ap()
    ident = nc.dram_tensor("ident", [128, 128], F32, kind="ExternalInput").ap()
    h = nc.dram_tensor("h", [T, D], F32, kind="ExternalOutput").ap()
    hT = nc.dram_tensor("hT", [D, T], BF16, kind="ExternalOutput").ap()
    P = Prog(nc)
    with contextlib.ExitStack() as es:
        A = Arena(nc, es, 12000)
        gt = A.f32([128, D]); bt = A.f32([128, D]); idt = A.f32([128, 128])
        xt = [A.f32([128, D]) for _ in range(2)]
        yt = [A.f32([128, D]) for _ in range(2)]
        hTt = [A.bf16([128, 8, 128]) for _ in range(2)]
        st = [A.f32([128, 2, 6]) for _ in range(2)]
        mv = [A.f32([128, 2]) for _ in range(2)]
        rs = [A.f32([128, 1]) for _ in range(2)]
        pts = [Tl(es.enter_context(nc.psum_tensor("pt%d" % i, [128, 4, 128], F32))[:], excl=True) for i in range(2)]
        P.add("sp", lambda e: e.dma_start(out=gt.ap, in_=g.partition_broadcast(128)), writes=[gt], dma=True)
        P.add("sp", lambda e: e.dma_start(out=bt.ap, in_=b.partition_broadcast(128)), writes=[bt], dma=True)
        P.add("sp", lambda e: e.dma_start(out=idt.ap, in_=ident), writes=[idt], dma=True)
        hTv = hT.rearrange("(c p) t -> p c t", p=128)
        for t in range(NT):
            i = t % 2
            P.add("sp", lambda e, t=t, i=i: e.dma_start(out=xt[i].ap, in_=x[t * 128:(t + 1) * 128, :]), writes=[xt[i]], dma=True)
            layer_norm_tile(P, xt[i], yt[i], gt, bt, st[i], mv[i], rs[i])
            P.final(P.add("sp", lambda e, t=t, i=i: e.dma_start(out=h[t * 128:(t + 1) * 128, :], in_=yt[i].ap), reads=[yt[i]], dma=True))
            transpose_tile(P, yt[i], idt, pts, dst_bf=hTt[i])
            P.final(P.add("sp", lambda e, t=t, i=i: e.dma_start(out=hTv[:, :, t * 128:(t + 1) * 128], in_=hTt[i].ap), reads=[hTt[i]], dma=True))
        P.emit()
    print("ops recorded", P.count)
    return nc


def build_k2(T, SB=1024, max_ops=None):
    nc = bass.Bass("TRN2", target_bir_lowering=False)
    NB = T // 512
    NT = T // 128
    NSB = T // SB
    dt = nc.dram_tensor
    h_in = dt("h_in", [T, D], F32, kind="ExternalInput").ap()
    yT_in = dt("yT_in", [D, T], BF16, kind="ExternalInput").ap()
    memT = dt("memT", [D, MEM], F32, kind="ExternalInput").ap()
    w_out = dt("w_out", [D, D], F32, kind="ExternalInput").ap()
    wq = dt("wq", [D, D], F32, kind="ExternalInput").ap()
    wk = dt("wk", [D, D], F32, kind="ExternalInput").ap()
    wv = dt("wv", [D, D], F32, kind="ExternalInput").ap()
    wo = dt("wo", [D, D], F32, kind="ExternalInput").ap()
    rw = dt("rw", [D, NEXP], F32, kind="ExternalInput").ap()
    rb = dt("rb", [1, NEXP], F32, kind="ExternalInput").ap()
    w1 = dt("w1", [NEXP, D, FF], F32, kind="ExternalInput").ap()
    w3 = dt("w3", [NEXP, D, FF], F32, kind="ExternalInput").ap()
    w2 = dt("w2", [NEXP, FF, D], F32, kind="ExternalInput").ap()
    lng = dt("lng", [3, D], F32, kind="ExternalInput").ap()
    lnb = dt("lnb", [3, D], F32, kind="ExternalInput").ap()
    ident = dt("ident", [128, 128], F32, kind="ExternalInput").ap()
    h_out = dt("h_out", [T, D], F32, kind="ExternalOutput").ap()
    hT_out = dt("hT_out", [D, T], BF16, kind="ExternalOutput").ap()
    h2_s = dt("h2_s", [T, D], F32).ap()
    h2T_s = dt("h2T_s", [D, T], BF16).ap()
    h2s_buf = [Buf("h2s%d" % i) for i in range(NT)]
    h2Ts_buf = [Buf("h2Ts%d" % i) for i in range(NT)]

    P = Prog(nc)
    P.max_ops = max_ops
    with contextlib.ExitStack() as es:
        A = Arena(nc, es, 53000)
        PO = Tl(es.enter_context(nc.psum_tensor("PO", [128, 1024], F32))[:], excl=True)
        PT = [Tl(es.enter_context(nc.psum_tensor("PT%d" % i, [128, 4, 128], F32))[:], excl=True) for i in range(2)]
        PX = [Tl(es.enter_context(nc.psum_tensor("PX%d" % i, [128, 512], F32))[:], excl=True) for i in range(4)]

        def dma(out_ap, in_ap, reads=(), writes=()):
            return P.add("sp", lambda e: e.dma_start(out=out_ap, in_=in_ap), reads=reads, writes=writes, dma=True)

        idt = A.f32([128, 128]); dma(idt.ap, ident, writes=[idt])
        gts = []
        for i in range(3):
            gt = A.f32([128, D]); bt = A.f32([128, D])
            dma(gt.ap, lng[i:i + 1, :].partition_broadcast(128), writes=[gt])
            dma(bt.ap, lnb[i:i + 1, :].partition_broadcast(128), writes=[bt])
            gts.append((gt, bt))
        rwt = A.f32([128, 8, NEXP]); dma(rwt.ap, rw.rearrange("(c p) n -> p c n", p=128), writes=[rwt])
        rbt = A.f32([128, NEXP]); dma(rbt.ap, rb.partition_broadcast(128), writes=[rbt])
        gates = A.f32([128, NT, NEXP])
        gate_buf = [Buf("gate%d" % i) for i in range(NT)]
        ones_bf = A.bf16([128, 128])
        P.add("pool", lambda e: e.memset(ones_bf.ap, 1.0), writes=[ones_bf])
        st = [A.f32([128, 2, 6]) for _ in range(2)]
        mv = [A.f32([128, 2]) for _ in range(2)]
        rs = [A.f32([128, 1]) for _ in range(2)]
        mark = A.off

        stg = [A.f32([128, 8, 256]) for _ in range(2)]
        stg_i = [0]

        def load_w(dst, src, KC, N, eng_cast=("pool", "dve")):
            srcv = src.rearrange("(c p) n -> p c n", p=128)
            for c0 in range(0, N, 256 * 8 // KC):
                cw = min(256 * 8 // KC, N - c0)
                s = stg[stg_i[0] % 2]
                ce = eng_cast[stg_i[0] % len(eng_cast)]
                stg_i[0] += 1
                sv = s.ap.rearrange("p a b -> p (a b)")[:, 0:KC * cw].rearrange("p (a b) -> p a b", b=cw)
                dma(sv, srcv[:, :, c0:c0 + cw], writes=[s])
                P.add(ce, lambda e, sv=sv, c0=c0, cw=cw: e.tensor_copy(out=dst[:, :, c0:c0 + cw], in_=sv), reads=[s], writes=[dst])

        wA = A.bf16([128, 8, D]); wB = A.bf16([128, 8, D]); wC = A.bf16([128, 8, D])
        memKT = A.bf16([128, 8, MEM]); memV = A.bf16([128, 2, D])
        memTb = A.bf16([128, 8, MEM])
        load_w(memTb, memT, 8, MEM)
        load_w(wB, wk, 8, D)
        load_w(wC, wv, 8, D)
        for c in range(8):
            px = PX[c % 2]

            def mm(e, c=c, px=px):
                ins = None
                for k in range(8):
                    ins = e.matmul(px[:, 0:MEM], lhsT=wB[:, k, c * 128:(c + 1) * 128], rhs=memTb[:, k, :], start=(k == 0), stop=(k == 7))
                return ins
            P.add("pe", mm, reads=[wB, memTb], writes=[px])
            P.add("act", lambda e, c=c, px=px: e.copy(out=memKT[:, c, :], in_=px[:, 0:MEM]), reads=[px], writes=[memKT])
        for kc in range(2):
            for hf in range(2):
                px = PX[2 + (kc * 2 + hf) % 2]

                def mm(e, kc=kc, hf=hf, px=px):
                    ins = None
                    for k in range(8):
                        ins = e.matmul(px.ap, lhsT=memTb[:, k, kc * 128:(kc + 1) * 128], rhs=wC[:, k, hf * 512:(hf + 1) * 512], start=(k == 0), stop=(k == 7))
                    return ins
                P.add("pe", mm, reads=[wC, memTb], writes=[px])
                P.add("act", lambda e, kc=kc, hf=hf, px=px: e.copy(out=memV[:, kc, hf * 512:(hf + 1) * 512], in_=px.ap), reads=[px], writes=[memV])
        w_outb, wqb, wob = wA, wB, wC
        load_w(w_outb, w_out, 8, D)
        load_w(wqb, wq, 8, D)
        load_w(wob, wo, 8, D)

        yTb = [A.bf16([128, 8, 512]) for _ in range(2)]
        ht = [A.f32([128, D]) for _ in range(2)]
        h1 = A.f32([128, 4, D])
        h1_t = [Tl(h1[:, i, :]) for i in range(4)]
        h1T = A.bf16([128, 8, 512])
        h1T_t = [Buf() for _ in range(4)]
        qT = A.bf16([128, 8, 512])
        pT = [A.bf16([128, 512]) for _ in range(2)]
        aT = A.bf16([128, 8, 512])
        rden = A.f32([128, 512])
        z2 = [A.f32([128, D]) for _ in range(2)]
        h2Tb = [A.bf16([128, 8, 128]) for _ in range(2)]
        h2Tf = [A.f32([128, 8, 128]) for _ in range(2)]
        gsc = A.f32([128, 16]); gbi = A.f32([128, 16]); geq1 = A.f32([128, 16]); gb2 = A.f32([128, 16])
        geq2 = A.f32([128, 16]); gm1 = A.f32([128, 4]); gm2 = A.f32([128, 4]); ggs = A.f32([128, 4])
        gmx = A.f32([128, 1]); ging = A.f32([128, 4]); gden = A.f32([128, 1])
        yTv = yT_in.rearrange("(c p) t -> p c t", p=128)
        h2Tv = h2T_s.rearrange("(c p) t -> p c t", p=128)
        g3 = lambda t: t.ap.rearrange("p (a b) -> p a b", b=4)

        for blk in range(NB):
            yb = yTb[blk % 2]
            dma(yb.ap, yTv[:, :, blk * 512:(blk + 1) * 512], writes=[yb])
            for tt in range(4):
                t = blk * 4 + tt
                hti = ht[t % 2]
                dma(hti.ap, h_in[t * 128:(t + 1) * 128, :], writes=[hti])

                def mm(e, tt=tt, yb=yb):
                    ins = None
                    for hf in range(2):
                        for k in range(8):
                            ins = e.matmul(PO[:, hf * 512:(hf + 1) * 512], lhsT=yb[:, k, tt * 128:(tt + 1) * 128],
                                           rhs=w_outb[:, k, hf * 512:(hf + 1) * 512], start=(k == 0), stop=(k == 7))
                    return ins
                P.add("pe", mm, reads=[yb, w_outb], writes=[PO])
                P.add("dve", lambda e, hti=hti: e.scalar_tensor_tensor(out=hti.ap, in0=hti.ap, scalar=ALPHA, in1=PO.ap, op0=ALU.mult, op1=ALU.add),
                      reads=[hti, PO], writes=[hti])
                layer_norm_tile(P, hti, h1_t[tt], gts[0][0], gts[0][1], st[t % 2], mv[t % 2], rs[t % 2])
                for hh in range(2):
                    pt = PT[hh]

                    def tr(e, hh=hh, pt=pt, tt=tt):
                        ins = None
                        for c in range(4):
                            ins = e.transpose(out=pt[:, c, :], in_=h1[:, tt, (hh * 4 + c) * 128:(hh * 4 + c + 1) * 128], identity=idt.ap)
                        return ins
                    P.add("pe", tr, reads=[h1_t[tt], idt], writes=[pt])
                    P.add("act", lambda e, hh=hh, pt=pt, tt=tt: e.copy(out=h1T[:, hh * 4:(hh + 1) * 4, tt * 128:(tt + 1) * 128], in_=pt.ap),
                          reads=[pt], writes=[h1T])
            for c in range(8):
                px = PX[c % 2]

                def mm(e, c=c, px=px):
                    ins = None
                    for k in range(8):
                        ins = e.matmul(px.ap, lhsT=wqb[:, k, c * 128:(c + 1) * 128], rhs=h1T[:, k, :], start=(k == 0), stop=(k == 7))
                    return ins
                P.add("pe", mm, reads=[wqb, h1T], writes=[px])
                P.add("act", lambda e, c=c, px=px: e.copy(out=qT[:, c, :], in_=px.ap), reads=[px], writes=[qT])
            for hd in range(4):
                for kc in range(2):
                    px = PX[kc]

                    def mm(e, hd=hd, kc=kc, px=px):
                        ins = None
                        for dc in range(2):
                            ins = e.matmul(px.ap, lhsT=memKT[:, 2 * hd + dc, kc * 128:(kc + 1) * 128], rhs=qT[:, 2 * hd + dc, :],
                                           start=(dc == 0), stop=(dc == 1))
                        return ins
                    P.add("pe", mm, reads=[memKT, qT], writes=[px])
                    P.add("act", lambda e, kc=kc, px=px: e.activation(out=pT[kc].ap, in_=px.ap, func=AF.Exp, scale=1.0 / 16.0),
                          reads=[px], writes=[pT[kc]])

                def mmd(e):
                    ins = None
                    for kc in range(2):
                        ins = e.matmul(PX[2].ap, lhsT=ones_bf.ap, rhs=pT[kc].ap, start=(kc == 0), stop=(kc == 1))
                    return ins
                P.add("pe", mmd, reads=[ones_bf, pT[0], pT[1]], writes=[PX[2]])
                P.add("dve", lambda e: e.reciprocal(out=rden.ap, in_=PX[2].ap), reads=[PX[2]], writes=[rden])
                for dvc in range(2):
                    def mmo(e, hd=hd, dvc=dvc):
                        ins = None
                        for kc in range(2):
                            ins = e.matmul(PX[3].ap, lhsT=memV[:, kc, (2 * hd + dvc) * 128:(2 * hd + dvc + 1) * 128], rhs=pT[kc].ap,
                                           start=(kc == 0), stop=(kc == 1))
                        return ins
                    P.add("pe", mmo, reads=[memV, pT[0], pT[1]], writes=[PX[3]])
                    P.add("dve", lambda e, hd=hd, dvc=dvc: e.tensor_tensor(out=aT[:, 2 * hd + dvc, :], in0=PX[3].ap, in1=rden.ap, op=ALU.mult),
                          reads=[PX[3], rden], writes=[aT])
            for tt in range(4):
                t = blk * 4 + tt
                z = z2[t % 2]

                def mm(e, tt=tt):
                    ins = None
                    for hf in range(2):
                        for k in range(8):
                            ins = e.matmul(PO[:, hf * 512:(hf + 1) * 512], lhsT=aT[:, k, tt * 128:(tt + 1) * 128],
                                           rhs=wob[:, k, hf * 512:(hf + 1) * 512], start=(k == 0), stop=(k == 7))
                    return ins
                P.add("pe", mm, reads=[aT, wob], writes=[PO])
                P.add("dve", lambda e, tt=tt, z=z: e.scalar_tensor_tensor(out=z.ap, in0=h1[:, tt, :], scalar=ALPHA, in1=PO.ap, op0=ALU.mult, op1=ALU.add),
                      reads=[h1_t[tt], PO], writes=[z])
                layer_norm_tile(P, z, z, gts[1][0], gts[1][1], st[t % 2], mv[t % 2], rs[t % 2])
                dma(h2_s[t * 128:(t + 1) * 128, :], z.ap, reads=[z], writes=[h2s_buf[t]])
                hb = h2Tb[t % 2]; hf32 = h2Tf[t % 2]
                transpose_tile(P, z, idt, PT, dst_bf=hb, dst_f32=hf32)
                dma(h2Tv[:, :, t * 128:(t + 1) * 128], hb.ap, reads=[hb], writes=[h2Ts_buf[t]])
                pr = PX[2]

                def mmr(e, hf32=hf32, pr=pr):
                    ins = None
                    for k in range(8):
                        ins = e.matmul(pr[:, 0:NEXP], lhsT=hf32[:, k, :], rhs=rwt[:, k, :], start=(k == 0), stop=(k == 7))
                    return ins
                P.add("pe", mmr, reads=[hf32, rwt], writes=[pr])
                P.add("act", lambda e, pr=pr: e.activation(out=gsc.ap, in_=pr[:, 0:NEXP], func=AF.Sigmoid), reads=[pr], writes=[gsc])
                V = lambda fn, r, w: P.add("dve", fn, reads=r, writes=w)
                V(lambda e: e.tensor_tensor(out=gbi.ap, in0=gsc.ap, in1=rbt.ap, op=ALU.add), [gsc, rbt], [gbi])
                V(lambda e: e.tensor_reduce(out=gm1.ap, in_=g3(gbi), axis=AX.X, op=ALU.max), [gbi], [gm1])
                V(lambda e: e.tensor_tensor(out=g3(geq1), in0=g3(gbi), in1=gm1.ap.unsqueeze(2).to_broadcast([128, 4, 4]), op=ALU.is_equal), [gbi, gm1], [geq1])
                V(lambda e: e.scalar_tensor_tensor(out=gb2.ap, in0=geq1.ap, scalar=-1e9, in1=gbi.ap, op0=ALU.mult, op1=ALU.add), [geq1, gbi], [gb2])
                V(lambda e: e.tensor_reduce(out=gm2.ap, in_=g3(gb2), axis=AX.X, op=ALU.max), [gb2], [gm2])
                V(lambda e: e.tensor_tensor(out=g3(geq2), in0=g3(gb2), in1=gm2.ap.unsqueeze(2).to_broadcast([128, 4, 4]), op=ALU.is_equal), [gb2, gm2], [geq2])
                V(lambda e: e.tensor_tensor(out=ggs.ap, in0=gm1.ap, in1=gm2.ap, op=ALU.add), [gm1, gm2], [ggs])
                V(lambda e: e.tensor_reduce(out=gmx.ap, in_=ggs.ap, axis=AX.X, op=ALU.max), [ggs], [gmx])
                V(lambda e: e.tensor_scalar(out=ging.ap, in0=ggs.ap, scalar1=gmx[:, 0:1], scalar2=None, op0=ALU.is_equal), [ggs, gmx], [ging])
                V(lambda e: e.tensor_tensor(out=geq1.ap, in0=geq1.ap, in1=geq2.ap, op=ALU.add), [geq1, geq2], [geq1])
                V(lambda e: e.tensor_tensor(out=g3(geq1), in0=g3(geq1), in1=ging.ap.unsqueeze(2).to_broadcast([128, 4, 4]), op=ALU.mult), [geq1, ging], [geq1])
                V(lambda e: e.tensor_tensor(out=geq1.ap, in0=geq1.ap, in1=gsc.ap, op=ALU.mult), [geq1, gsc], [geq1])
                V(lambda e: e.tensor_reduce(out=gden.ap, in_=geq1.ap, axis=AX.X, op=ALU.add), [geq1], [gden])
                V(lambda e: e.reciprocal(out=gden.ap, in_=gden.ap), [gden], [gden])
                V(lambda e, t=t: e.tensor_scalar(out=gates[:, t, :], in0=geq1.ap, scalar1=gden[:, 0:1], scalar2=None, op0=ALU.mult), [geq1, gden], [gate_buf[t]])

        P.barrier()
        A.off = mark
        stg2 = [A.f32([128, 8, 256]) for _ in range(2)]
        stg[0], stg[1] = stg2[0], stg2[1]
        h2Tsb = A.bf16([128, 8, SB])
        acc = A.f32([128, SB // 128, D])
        acc_t = [Tl(acc[:, i, :]) for i in range(SB // 128)]
        wset = [(A.bf16([128, 8, FF]), A.bf16([128, 8, FF]), A.bf16([128, 4, D])) for _ in range(2)]
        hid = [A.bf16([128, 4, 512]) for _ in range(2)]
        sl = [A.f32([128, 512]) for _ in range(2)]
        h2t = [A.f32([128, D]) for _ in range(2)]
        hTt = [A.bf16([128, 8, 128]) for _ in range(2)]
        hTov = hT_out.rearrange("(c p) t -> p c t", p=128)
        cnt = 0
        for sb in range(NSB):
            t0 = sb * (SB // 128)
            dma(h2Tsb.ap, h2Tv[:, :, sb * SB:(sb + 1) * SB], reads=h2Ts_buf[t0:t0 + SB // 128], writes=[h2Tsb])
            for ex in range(NEXP):
                w1b, w3b, w2b = wset[ex % 2]
                load_w(w1b, w1[ex], 8, FF)
                load_w(w3b, w3[ex], 8, FF)
                load_w(w2b, w2[ex], 4, D)
                for b4 in range(SB // 512):
                    hd_ = hid[cnt % 2]
                    for fc in range(4):
                        p1 = PX[fc % 2]; p3 = PX[2 + fc % 2]; s_ = sl[fc % 2]

                        def mm1(e, fc=fc, p1=p1, w1b=w1b, b4=b4):
                            ins = None
                            for k in range(8):
                                ins = e.matmul(p1.ap, lhsT=w1b[:, k, fc * 128:(fc + 1) * 128], rhs=h2Tsb[:, k, b4 * 512:(b4 + 1) * 512], start=(k == 0), stop=(k == 7))
                            return ins

                        def mm3(e, fc=fc, p3=p3, w3b=w3b, b4=b4):
                            ins = None
                            for k in range(8):
                                ins = e.matmul(p3.ap, lhsT=w3b[:, k, fc * 128:(fc + 1) * 128], rhs=h2Tsb[:, k, b4 * 512:(b4 + 1) * 512], start=(k == 0), stop=(k == 7))
                            return ins
                        P.add("pe", mm1, reads=[w1b, h2Tsb], writes=[p1])
                        P.add("pe", mm3, reads=[w3b, h2Tsb], writes=[p3])
                        P.add("act", lambda e, p1=p1, s_=s_: e.activation(out=s_.ap, in_=p1.ap, func=AF.Silu), reads=[p1], writes=[s_])
                        P.add("dve", lambda e, p3=p3, s_=s_, fc=fc, hd_=hd_: e.tensor_tensor(out=hd_[:, fc, :], in0=p3.ap, in1=s_.ap, op=ALU.mult),
                              reads=[p3, s_], writes=[hd_])
                    for tt in range(4):
                        ti = b4 * 4 + tt

                        def mm2(e, tt=tt, hd_=hd_, w2b=w2b):
                            ins = None
                            for hf in range(2):
                                for fc in range(4):
                                    ins = e.matmul(PO[:, hf * 512:(hf + 1) * 512], lhsT=hd_[:, fc, tt * 128:(tt + 1) * 128],
                                                   rhs=w2b[:, fc, hf * 512:(hf + 1) * 512], start=(fc == 0), stop=(fc == 3))
                            return ins
                        P.add("pe", mm2, reads=[hd_, w2b], writes=[PO])
                        gcol = gates[:, t0 + ti, ex:ex + 1]
                        if ex == 0:
                            P.add("dve", lambda e, ti=ti, gcol=gcol: e.tensor_scalar(out=acc[:, ti, :], in0=PO.ap, scalar1=gcol, scalar2=None, op0=ALU.mult),
                                  reads=[PO, gate_buf[t0 + ti]], writes=[acc_t[ti]])
                        else:
                            P.add("dve", lambda e, ti=ti, gcol=gcol: e.scalar_tensor_tensor(out=acc[:, ti, :], in0=PO.ap, scalar=gcol, in1=acc[:, ti, :],
                                                                                             op0=ALU.mult, op1=ALU.add),
                                  reads=[PO, gate_buf[t0 + ti], acc_t[ti]], writes=[acc_t[ti]])
                    cnt += 1
            for ti in range(SB // 128):
                t = t0 + ti
                hx = h2t[t % 2]
                dma(hx.ap, h2_s[t * 128:(t + 1) * 128, :], reads=[h2s_buf[t]], writes=[hx])
                P.add("dve", lambda e, hx=hx, ti=ti: e.scalar_tensor_tensor(out=hx.ap, in0=hx.ap, scalar=ALPHA, in1=acc[:, ti, :], op0=ALU.mult, op1=ALU.add),
                      reads=[hx, acc_t[ti]], writes=[hx])
                layer_norm_tile(P, hx, hx, gts[2][0], gts[2][1], st[t % 2], mv[t % 2], rs[t % 2])
                P.final(dma(h_out[t * 128:(t + 1) * 128, :], hx.ap, reads=[hx]))
                transpose_tile(P, hx, idt, PT, dst_bf=hTt[t % 2])
                P.final(dma(hTov[:, :, t * 128:(t + 1) * 128], hTt[t % 2].ap, reads=[hTt[t % 2]]))
        P.emit()
    print("ops recorded", P.count)
    return nc


def build_k1(S, layer, max_ops=None):
    nc = bass.Bass("TRN2", target_bir_lowering=False)
    NJ = S // 512
    lam_init = 0.8 - 0.6 * math.exp(-0.3 * layer)
    dt = nc.dram_tensor
    I = lambda name, shape, d=F32: dt(name, shape, d, kind="ExternalInput").ap()
    hT = I("hT", [D, S], BF16)
    wfq = I("wfq", [2, D, 70]); wfk = I("wfk", [2, D, 70]); wfv = I("wfv", [D, 128]); wff = I("wff", [D, 44])
    wdq = I("wdq", [2, D, 64]); wdk = I("wdk", [2, D, 64]); wdv = I("wdv", [D, 128])
    whq = I("whq", [D, 256]); whf = I("whf", [D, 256]); whi = I("whi", [D, 256]); whg = I("whg", [D, 256])
    fbr = I("fbr", [44, 1]); cst = I("cst", [44, 8]); sel = I("sel", [44, 4, 70])
    maskF = I("maskF", [128, 4, 512]); maskD = I("maskD", [128, 4, 512]); maskH = I("maskH", [64, 64])
    ident = I("ident", [128, 128])
    lamp = I("lamp", [4, 32]); dng = I("dng", [64, 1]); hlb = I("hlb", [128, 2, 2]); hng = I("hng", [1, 128])
    O = lambda name, shape, d=BF16: dt(name, shape, d, kind="ExternalOutput").ap()
    yf = O("yf", [128, S]); yd = O("yd", [128, S]); yh = O("yh", [S, 256])

    P = Prog(nc)
    P.max_ops = max_ops
    with contextlib.ExitStack() as es:
        A = Arena(nc, es, 53000)
        PS_ = lambda name, shape, d=F32: Tl(es.enter_context(nc.psum_tensor(name, shape, d))[:], excl=True)
        PP = [PS_("PP%d" % i, [128, 512]) for i in range(2)]
        PSc = [PS_("PS%d" % i, [128, 512]) for i in range(2)]
        PA = PS_("PA", [128, 512]); PB = PS_("PB", [128, 512])
        PM0 = PS_("PM0", [128, 512]); PM1 = PS_("PM1", [128, 512])

        def dma(out_ap, in_ap, reads=(), writes=()):
            return P.add("sp", lambda e: e.dma_start(out=out_ap, in_=in_ap), reads=reads, writes=writes, dma=True)
        V = lambda fn, r, w: P.add("dve", fn, reads=r, writes=w)
        AC = lambda fn, r, w: P.add("act", fn, reads=r, writes=w)
        PE = lambda fn, r, w: P.add("pe", fn, reads=r, writes=w)

        def const_f32(shape, src):
            t = A.f32(shape)
            dma(t.ap, src, writes=[t])
            return t
        fbt = const_f32([44, 1], fbr); cstt = const_f32([44, 8], cst)
        mH = const_f32([64, 64], maskH)
        dngt = const_f32([64, 1], dng)
        hngt = A.f32([64, 128]); dma(hngt.ap, hng.partition_broadcast(64), writes=[hngt])
        onesf = A.f32([128, 64]); P.add("pool", lambda e: e.memset(onesf.ap, 1.0), writes=[onesf])
        onesb = A.bf16([128, 64]); P.add("pool", lambda e: e.memset(onesb.ap, 1.0), writes=[onesb])
        lamt = A.f32([64, 4, 32])
        for i in range(4):
            dma(lamt[:, i, :], lamp[i:i + 1, :].partition_broadcast(64), writes=[lamt])
        lpr = A.f32([64, 2, 32]); lsum = A.f32([64, 2]); nlam = A.f32([64, 1])
        V(lambda e: e.tensor_tensor(out=lpr[:, 0, :], in0=lamt[:, 0, :], in1=lamt[:, 1, :], op=ALU.mult), [lamt], [lpr])
        V(lambda e: e.tensor_tensor(out=lpr[:, 1, :], in0=lamt[:, 2, :], in1=lamt[:, 3, :], op=ALU.mult), [lamt, lpr], [lpr])
        V(lambda e: e.tensor_reduce(out=lsum.ap, in_=lpr.ap, axis=AX.X, op=ALU.add), [lpr], [lsum])
        AC(lambda e: e.activation(out=lsum.ap, in_=lsum.ap, func=AF.Exp), [lsum], [lsum])
        V(lambda e: e.tensor_tensor(out=nlam.ap, in0=lsum[:, 1:2], in1=lsum[:, 0:1], op=ALU.subtract), [lsum], [nlam])
        V(lambda e: e.tensor_scalar_add(out=nlam.ap, in0=nlam.ap, scalar1=-lam_init), [nlam], [nlam])
        lbt = A.f32([128, 2]); omlb = A.f32([128, 2])
        if layer > 0:
            hlbt = const_f32([128, 2, 2], hlb)
            V(lambda e: e.tensor_tensor(out=lbt.ap, in0=hlbt[:, 1, :], in1=hlbt[:, 0, :], op=ALU.subtract), [hlbt], [lbt])
            AC(lambda e: e.activation(out=lbt.ap, in_=lbt.ap, func=AF.Sigmoid), [lbt], [lbt])
            V(lambda e: e.tensor_scalar(out=omlb.ap, in0=lbt.ap, scalar1=-1.0, scalar2=1.0, op0=ALU.mult, op1=ALU.add), [lbt], [omlb])

        Wfq = [A.bf16([128, 8, 70]) for _ in range(2)]; Wfk = [A.bf16([128, 8, 70]) for _ in range(2)]
        Wfv = A.bf16([128, 8, 128]); Wff = A.bf16([128, 8, 44])
        Wdq = [A.bf16([128, 8, 64]) for _ in range(2)]; Wdk = [A.bf16([128, 8, 64]) for _ in range(2)]
        Wdv = A.bf16([128, 8, 128])
        Whq = A.bf16([128, 8, 256]); Whf = A.bf16([128, 8, 256]); Whi = A.bf16([128, 8, 256]); Whg = A.bf16([128, 8, 256])
        selb = A.bf16([44, 4, 70]); mFb = A.bf16([128, 4, 512]); mDb = A.bf16([128, 4, 512]); idb = A.bf16([128, 128])
        kaug = [A.bf16([70, S]) for _ in range(2)]
        kaug_b = [[Buf() for _ in range(NJ)] for _ in range(2)]
        fva = [A.bf16([128, S // 128, 65]) for _ in range(2)]
        fva_b = [[Buf() for _ in range(NJ)] for _ in range(2)]
        dk = [A.bf16([64, S]) for _ in range(2)]
        dk_b = [[Buf() for _ in range(NJ)] for _ in range(2)]
        dva = [A.bf16([128, S // 128, 65]) for _ in range(2)]
        dva_b = [[Buf() for _ in range(NJ)] for _ in range(2)]
        for t_ in fva + dva:
            P.add("pool", lambda e, t_=t_: e.memset(t_[:, :, 64:65], 1.0), writes=[t_] )
        Sst = [A.f32([128, 128]) for _ in range(2)]
        for t_ in Sst:
            P.add("pool", lambda e, t_=t_: e.memset(t_.ap, 0.0), writes=[t_])
        carry = A.f32([44, 1]); P.add("pool", lambda e: e.memset(carry.ap, 0.0), writes=[carry])
        mark = A.off
        stg = [A.f32([128, 8, 256]) for _ in range(2)]
        stg_i = [0]

        def load_w(dst, src_v, shape, rows=128):
            a, b = shape
            s = stg[stg_i[0] % 2]
            ce = ("pool", "dve")[stg_i[0] % 2]
            stg_i[0] += 1
            sv = s.ap.rearrange("p a b -> p (a b)")[0:rows, 0:a * b].rearrange("p (a b) -> p a b", b=b)
            dma(sv, src_v, writes=[s])
            P.add(ce, lambda e: e.tensor_copy(out=dst.ap, in_=sv), reads=[s], writes=[dst])
        kv = lambda w: w.rearrange("(c p) n -> p c n", p=128)
        for hd in range(2):
            load_w(Wfq[hd], kv(wfq[hd]), (8, 70)); load_w(Wfk[hd], kv(wfk[hd]), (8, 70))
            load_w(Wdq[hd], kv(wdq[hd]), (8, 64)); load_w(Wdk[hd], kv(wdk[hd]), (8, 64))
        load_w(Wfv, kv(wfv), (8, 128)); load_w(Wff, kv(wff), (8, 44)); load_w(Wdv, kv(wdv), (8, 128))
        load_w(Whq, kv(whq), (8, 256)); load_w(Whf, kv(whf), (8, 256)); load_w(Whi, kv(whi), (8, 256)); load_w(Whg, kv(whg), (8, 256))
        load_w(selb, sel, (4, 70), rows=44)
        load_w(mFb, maskF, (4, 512)); load_w(mDb, maskD, (4, 512))
        idf = stg[stg_i[0] % 2]
        dma(idf.ap.rearrange("p a b -> p (a b)")[:, 0:128], ident, writes=[idf])
        P.add("dve", lambda e: e.tensor_copy(out=idb.ap, in_=idf.ap.rearrange("p a b -> p (a b)")[:, 0:128]), reads=[idf], writes=[idb])
        P.barrier()
        A.off = mark

        hTb = [A.bf16([128, 8, 512]) for _ in range(2)]
        SC = [A.f32([128, 512]) for _ in range(9)]
        BFt = [A.bf16([128, 512]) for _ in range(4)]
        Rb = A.bf16([44, 512])
        qa = [A.bf16([70, 512]) for _ in range(2)]
        qd = [A.bf16([64, 512]) for _ in range(2)]
        pT = [A.bf16([128, 512]) for _ in range(4)]
        yo = [A.bf16([64, 512]) for _ in range(2)]
        QD = A.bf16([128, 512]); KD = A.bf16([128, 512])
        sm = A.f32([128, 8]); bl = A.f32([128, 8]); em = A.f32([128, 8]); el = A.f32([128, 8]); eb = A.f32([128, 8])
        vtok = A.bf16([64, 8, 256]); gtok = A.bf16([64, 8, 256]); yst = A.bf16([64, 8, 256])
        scm = A.bf16([64, 64]); KT = A.bf16([64, 128]); SPb = A.bf16([128, 128])
        tmpS = A.f32([128, 128]); sq = A.f32([64, 128]); t1 = A.f32([64, 128]); ss = A.f32([64, 1])
        hTv = hT.rearrange("(c p) t -> p c t", p=128)
        yhv = yh.rearrange("(c p) n -> p c n", p=64)
        pcnt = [0]

        def proj(M, wsel, hb, tok=None, extra=None):
            pp = PP[pcnt[0] % 2]
            pcnt[0] += 1
            W, c0 = wsel

            def mm(e):
                ins = None
                for k in range(8):
                    last = (k == 7) and extra is None
                    if tok is None:
                        ins = e.matmul(pp[0:M, :], lhsT=W[:, k, c0:c0 + M], rhs=hb[:, k, :], start=(k == 0), stop=last)
                    else:
                        ins = e.matmul(pp[0:tok[1], 0:M], lhsT=hb[:, k, tok[0]:tok[0] + tok[1]], rhs=W[:, k, c0:c0 + M], start=(k == 0), stop=last)
                if extra is not None:
                    ins = e.matmul(pp[0:M, :], lhsT=extra[0], rhs=extra[1], start=False, stop=True)
                return ins
            rd = [W, hb] + ([extra[2], extra[3]] if extra is not None else [])
            PE(mm, rd, [pp])
            return pp

        def block_body(j):
            hb = hTb[j % 2]
            dma(hb.ap, hTv[:, :, j * 512:(j + 1) * 512], writes=[hb])
            c0, c1 = j * 512, (j + 1) * 512
            pp = proj(44, (Wff, 0), hb)
            F = [Tl(SC[i][0:44, :], buf=SC[i].buf) for i in range(9)]
            Bq = [Tl(BFt[i][0:44, :], buf=BFt[i].buf) for i in range(4)]
            AC(lambda e, pp=pp: e.activation(out=F[0].ap, in_=pp[0:44, :], func=AF.Sigmoid, bias=fbt.ap), [pp, fbt], [F[0]])
            AC(lambda e: e.activation(out=F[1].ap, in_=F[0].ap, func=AF.Ln), [F[0]], [F[1]])
            src, dst = F[1], F[2]
            s_ = 1
            while s_ < 512:
                V(lambda e, src=src, dst=dst, s_=s_: e.tensor_copy(out=dst[:, 0:s_], in_=src[:, 0:s_]), [src], [dst])
                V(lambda e, src=src, dst=dst, s_=s_: e.tensor_tensor(out=dst[:, s_:512], in0=src[:, s_:512], in1=src[:, 0:512 - s_], op=ALU.add), [src, dst], [dst])
                src, dst = dst, src
                s_ *= 2
            cs = src
            cum = F[3]
            V(lambda e, cs=cs: e.tensor_scalar(out=cum.ap, in0=cs.ap, scalar1=carry[:, 0:1], scalar2=None, op0=ALU.add), [cs, carry], [cum])
            V(lambda e: e.tensor_copy(out=carry.ap, in_=cum[:, 511:512]), [cum], [carry])
            hf, mf, lf_, r1, r2, comb = F[4], F[5], F[6], F[7], F[8], F[0]
            V(lambda e: e.tensor_copy(out=Bq[0].ap, in_=cum.ap), [cum], [Bq[0]])
            V(lambda e: e.tensor_copy(out=hf.ap, in_=Bq[0].ap), [Bq[0]], [hf])
            V(lambda e: e.tensor_tensor(out=r1.ap, in0=cum.ap, in1=hf.ap, op=ALU.subtract), [cum, hf], [r1])
            V(lambda e: e.tensor_copy(out=Bq[1].ap, in_=r1.ap), [r1], [Bq[1]])
            V(lambda e: e.tensor_copy(out=mf.ap, in_=Bq[1].ap), [Bq[1]], [mf])
            V(lambda e: e.tensor_tensor(out=r2.ap, in0=r1.ap, in1=mf.ap, op=ALU.subtract), [r1, mf], [r2])
            V(lambda e: e.tensor_copy(out=Bq[2].ap, in_=r2.ap), [r2], [Bq[2]])
            V(lambda e: e.tensor_copy(out=lf_.ap, in_=Bq[2].ap), [Bq[2]], [lf_])
            V(lambda e: e.tensor_scalar(out=comb.ap, in0=hf.ap, scalar1=cstt[:, 0:1], scalar2=None, op0=ALU.mult), [hf, cstt], [comb])
            V(lambda e: e.scalar_tensor_tensor(out=comb.ap, in0=mf.ap, scalar=cstt[:, 1:2], in1=comb.ap, op0=ALU.mult, op1=ALU.add), [mf, cstt, comb], [comb])
            V(lambda e: e.scalar_tensor_tensor(out=comb.ap, in0=lf_.ap, scalar=cstt[:, 2:3], in1=comb.ap, op0=ALU.mult, op1=ALU.add), [lf_, cstt, comb], [comb])
            V(lambda e: e.tensor_scalar(out=Rb.ap, in0=comb.ap, scalar1=cstt[:, 3:4], scalar2=cstt[:, 4:5], op0=ALU.mult, op1=ALU.add), [comb, cstt], [Rb])

            for hd in range(2):
                pp = proj(70, (Wfq[hd], 0), hb, extra=(selb[:, 2 * hd, :], Rb.ap, selb, Rb))
                AC(lambda e, pp=pp, hd=hd: e.copy(out=qa[hd].ap, in_=pp[0:70, :]), [pp], [qa[hd]])
                pp = proj(70, (Wfk[hd], 0), hb, extra=(selb[:, 2 * hd + 1, :], Rb.ap, selb, Rb))
                AC(lambda e, pp=pp, hd=hd: e.copy(out=kaug[hd][:, c0:c1], in_=pp[0:70, :]), [pp], [kaug_b[hd][j]])
                pp = proj(64, (Wdq[hd], 0), hb)
                AC(lambda e, pp=pp, hd=hd: e.copy(out=qd[hd].ap, in_=pp[0:64, :]), [pp], [qd[hd]])
                pp = proj(64, (Wdk[hd], 0), hb)
                AC(lambda e, pp=pp, hd=hd: e.copy(out=dk[hd][:, c0:c1], in_=pp[0:64, :]), [pp], [dk_b[hd][j]])
            for tt in range(4):
                pp = proj(128, (Wfv, 0), hb, tok=(tt * 128, 128))
                for hd in range(2):
                    AC(lambda e, pp=pp, hd=hd, tt=tt: e.copy(out=fva[hd][:, j * 4 + tt, 0:64], in_=pp[:, hd * 64:(hd + 1) * 64]), [pp], [fva_b[hd][j]])
                pp = proj(128, (Wdv, 0), hb, tok=(tt * 128, 128))
                for hd in range(2):
                    AC(lambda e, pp=pp, hd=hd, tt=tt: e.copy(out=dva[hd][:, j * 4 + tt, 0:64], in_=pp[:, hd * 64:(hd + 1) * 64]), [pp], [dva_b[hd][j]])

            nkb = 4 * (j + 1)
            cnt = [0]

            def sweep(maps, vat, vat_b, scale, mask):
                items = [(kb, m) for kb in range(nkb) for m in range(len(maps))]
                pend = []

                def score(kb, m):
                    kt, kbufs, r0, nr, qt, acc = maps[m]
                    ps = PSc[cnt[0] % 2]; pt = pT[cnt[0] % 4]
                    cnt[0] += 1
                    diag = kb >= 4 * j

                    def mm(e):
                        ins = e.matmul(ps.ap, lhsT=kt[r0:r0 + nr, kb * 128:(kb + 1) * 128], rhs=qt[r0:r0 + nr, :], start=True, stop=not diag)
                        if diag:
                            ins = e.matmul(ps.ap, lhsT=idb.ap, rhs=mask[:, kb - 4 * j, :], start=False, stop=True)
                        return ins
                    PE(mm, [kbufs[kb // 4], qt, idb, mask], [ps])
                    AC(lambda e: e.activation(out=pt.ap, in_=ps.ap, func=AF.Exp, scale=scale), [ps], [pt])
                    return pt

                def pv(kb, m, pt):
                    acc = maps[m][5]
                    PE(lambda e: e.matmul(acc[0:65, :], lhsT=vat[:, kb, 0:65], rhs=pt.ap, start=(kb == 0), stop=(kb == nkb - 1)),
                       [vat_b[kb // 4], pt], [acc])
                for idx, (kb, m) in enumerate(items):
                    pend.append((kb, m, score(kb, m)))
                    if len(pend) > 2:
                        pv(*pend.pop(0))
                while pend:
                    pv(*pend.pop(0))

            def normalize(acc, dst):
                rdt = SC[8]
                V(lambda e: e.reciprocal(out=rdt[64:65, :], in_=acc[64:65, :]), [acc], [rdt])
                PE(lambda e: e.matmul(PM0[0:64, :], lhsT=onesf[64:65, 0:64], rhs=rdt[64:65, :], start=True, stop=True), [onesf, rdt], [PM0])
                AC(lambda e: e.copy(out=dst[0:64, :], in_=acc[0:64, :]), [acc], [dst])
                V(lambda e: e.tensor_tensor(out=dst[0:64, :], in0=dst[0:64, :], in1=PM0[0:64, :], op=ALU.mult), [dst, PM0], [dst])

            for hd in range(2):
                sweep([(kaug[hd], kaug_b[hd], 0, 70, qa[hd], PA)], fva[hd], fva_b[hd], 0.125, mFb)
                normalize(PA, SC[0])
                y_ = yo[hd]
                V(lambda e, y_=y_: e.tensor_copy(out=y_.ap, in_=SC[0][0:64, :]), [SC[0]], [y_])
                P.final(dma(yf[hd * 64:(hd + 1) * 64, c0:c1], y_.ap, reads=[y_]))
            for hd in range(2):
                sweep([(dk[hd], dk_b[hd], 0, 32, qd[hd], PA), (dk[hd], dk_b[hd], 32, 32, qd[hd], PB)], dva[hd], dva_b[hd], 32 ** -0.5, mDb)
                normalize(PA, SC[0])
                normalize(PB, SC[1])
                y1, y2, sqb = SC[0], SC[1], BFt[0]
                V(lambda e: e.scalar_tensor_tensor(out=y1[0:64, :], in0=y2[0:64, :], scalar=nlam[:, 0:1], in1=y1[0:64, :], op0=ALU.mult, op1=ALU.add),
                  [y1, y2, nlam], [y1])
                V(lambda e: e.tensor_tensor(out=sqb[0:64, :], in0=y1[0:64, :], in1=y1[0:64, :], op=ALU.mult), [y1], [sqb])
                PE(lambda e: e.matmul(PM0[0:64, :], lhsT=onesb[0:64, 0:64], rhs=sqb[0:64, :], start=True, stop=True), [onesb, sqb], [PM0])
                V(lambda e: e.tensor_scalar(out=y2[0:64, :], in0=PM0[0:64, :], scalar1=1.0 / 64.0, scalar2=EPS, op0=ALU.mult, op1=ALU.add), [PM0], [y2])
                AC(lambda e: e.sqrt(out=y2[0:64, :], in_=y2[0:64, :]), [y2], [y2])
                V(lambda e: e.reciprocal(out=y2[0:64, :], in_=y2[0:64, :]), [y2], [y2])
                V(lambda e: e.tensor_tensor(out=y1[0:64, :], in0=y1[0:64, :], in1=y2[0:64, :], op=ALU.mult), [y1, y2], [y1])
                y_ = yo[hd]
                V(lambda e, y_=y_: e.tensor_scalar(out=y_.ap, in0=y1[0:64, :], scalar1=dngt[:, 0:1], scalar2=1.0 - lam_init, op0=ALU.mult, op1=ALU.mult),
                  [y1, dngt], [y_])
                P.final(dma(yd[hd * 64:(hd + 1) * 64, c0:c1], y_.ap, reads=[y_]))

            for c in range(8):
                pp = proj(256, (Whi, 0), hb, tok=(c * 64, 64))
                AC(lambda e, pp=pp, c=c: e.copy(out=vtok[:, c, :], in_=pp[0:64, 0:256]), [pp], [vtok])
                pp = proj(256, (Whg, 0), hb, tok=(c * 64, 64))
                AC(lambda e, pp=pp, c=c: e.activation(out=gtok[:, c, :], in_=pp[0:64, 0:256], func=AF.Silu), [pp], [gtok])
            for hd in range(2):
                Sq, Sf, Sl, Sb, Sk, Sa, Si = SC[0], SC[1], SC[2], SC[3], SC[4], SC[5], SC[6]
                pp = proj(128, (Whq, hd * 128), hb)
                AC(lambda e, pp=pp: e.activation(out=Sq.ap, in_=pp.ap, func=AF.Silu), [pp], [Sq])
                pp = proj(128, (Whf, hd * 128), hb)
                AC(lambda e, pp=pp: e.activation(out=Sf.ap, in_=pp.ap, func=AF.Sigmoid), [pp], [Sf])
                if layer > 0:
                    V(lambda e, hd=hd: e.tensor_scalar(out=Sf.ap, in0=Sf.ap, scalar1=omlb[:, hd:hd + 1], scalar2=lbt[:, hd:hd + 1], op0=ALU.mult, op1=ALU.add),
                      [Sf, omlb, lbt], [Sf])
                AC(lambda e: e.activation(out=Sl.ap, in_=Sf.ap, func=AF.Ln), [Sf], [Sl])
                V(lambda e: e.tensor_scalar(out=Sk.ap, in0=Sf.ap, scalar1=-1.0, scalar2=1.0, op0=ALU.mult, op1=ALU.add), [Sf], [Sk])
                v3 = lambda t: t.ap.rearrange("p (a b) -> p a b", b=64)
                src, dst = Sl, Sb
                s_ = 1
                while s_ < 64:
                    V(lambda e, src=src, dst=dst, s_=s_: e.tensor_copy(out=v3(dst)[:, :, 0:s_], in_=v3(src)[:, :, 0:s_]), [src], [dst])
                    V(lambda e, src=src, dst=dst, s_=s_: e.tensor_tensor(out=v3(dst)[:, :, s_:64], in0=v3(src)[:, :, s_:64], in1=v3(src)[:, :, 0:64 - s_], op=ALU.add), [src, dst], [dst])
                    src, dst = dst, src
                    s_ *= 2
                bc, d1 = src, dst
                V(lambda e: e.tensor_copy(out=sm.ap, in_=v3(bc)[:, :, 31]), [bc], [sm])
                V(lambda e: e.tensor_copy(out=bl.ap, in_=v3(bc)[:, :, 63]), [bc], [bl])
                V(lambda e: e.tensor_tensor(out=v3(d1), in0=v3(bc), in1=sm.ap.unsqueeze(2).to_broadcast([128, 8, 64]), op=ALU.subtract), [bc, sm], [d1])
                AC(lambda e: e.activation(out=Sa.ap, in_=d1.ap, func=AF.Exp), [d1], [Sa])
                AC(lambda e: e.activation(out=Si.ap, in_=d1.ap, func=AF.Exp, scale=-1.0), [d1], [Si])
                V(lambda e: e.tensor_tensor(out=QD.ap, in0=Sq.ap, in1=Sa.ap, op=ALU.mult), [Sq, Sa], [QD])
                V(lambda e: e.tensor_tensor(out=KD.ap, in0=Sk.ap, in1=Si.ap, op=ALU.mult), [Sk, Si], [KD])
                AC(lambda e: e.activation(out=em.ap, in_=sm.ap, func=AF.Exp), [sm], [em])
                AC(lambda e: e.activation(out=eb.ap, in_=bl.ap, func=AF.Exp), [bl], [eb])
                V(lambda e: e.tensor_tensor(out=el.ap, in0=bl.ap, in1=sm.ap, op=ALU.subtract), [bl, sm], [el])
                AC(lambda e: e.activation(out=el.ap, in_=el.ap, func=AF.Exp), [el], [el])
                St = Sst[hd]
                kdT_ps = PM1[0:64, 128:192].bitcast(BF16)
                for c in range(8):
                    cs_ = slice(c * 64, (c + 1) * 64)
                    vt = vtok[:, c, hd * 128:(hd + 1) * 128]
                    PE(lambda e, cs_=cs_: e.matmul(PM0[0:64, 0:64], lhsT=KD[:, cs_], rhs=QD[:, cs_], start=True, stop=True), [KD, QD], [PM0])
                    V(lambda e: e.tensor_tensor(out=scm.ap, in0=PM0[0:64, 0:64], in1=mH.ap, op=ALU.mult), [PM0, mH], [scm])
                    PE(lambda e, cs_=cs_: e.transpose(out=kdT_ps, in_=KD[:, cs_], identity=idb.ap), [KD, idb], [PM1])
                    AC(lambda e: e.copy(out=KT.ap, in_=kdT_ps), [PM1], [KT])
                    V(lambda e, c=c, St=St: e.tensor_scalar(out=SPb.ap, in0=St.ap, scalar1=em[:, c:c + 1], scalar2=None, op0=ALU.mult), [St, em], [SPb])

                    def mmo(e, cs_=cs_, vt=vt):
                        e.matmul(PM0[0:64, 64:192], lhsT=scm.ap, rhs=vt, start=True, stop=False)
                        return e.matmul(PM0[0:64, 64:192], lhsT=QD[:, cs_], rhs=SPb.ap, start=False, stop=True)
                    PE(mmo, [scm, vtok, QD, SPb], [PM0])
                    PE(lambda e, vt=vt: e.matmul(PM1[:, 0:128], lhsT=KT.ap, rhs=vt, start=True, stop=True), [KT, vtok], [PM1])
                    V(lambda e, c=c: e.tensor_scalar(out=tmpS.ap, in0=PM1[:, 0:128], scalar1=el[:, c:c + 1], scalar2=None, op0=ALU.mult), [PM1, el], [tmpS])
                    V(lambda e, c=c, St=St: e.scalar_tensor_tensor(out=St.ap, in0=St.ap, scalar=eb[:, c:c + 1], in1=tmpS.ap, op0=ALU.mult, op1=ALU.add),
                      [St, eb, tmpS], [St])
                    AC(lambda e: e.activation(out=sq.ap, in_=PM0[0:64, 64:192], func=AF.Square), [PM0], [sq])
                    V(lambda e: e.tensor_reduce(out=ss.ap, in_=sq.ap, axis=AX.X, op=ALU.add), [sq], [ss])
                    V(lambda e: e.tensor_scalar(out=ss.ap, in0=ss.ap, scalar1=1.0 / 128.0, scalar2=EPS, op0=ALU.mult, op1=ALU.add), [ss], [ss])
                    AC(lambda e: e.sqrt(out=ss.ap, in_=ss.ap), [ss], [ss])
                    V(lambda e: e.reciprocal(out=ss.ap, in_=ss.ap), [ss], [ss])
                    V(lambda e: e.tensor_scalar(out=t1.ap, in0=PM0[0:64, 64:192], scalar1=ss[:, 0:1], scalar2=None, op0=ALU.mult), [PM0, ss], [t1])
                    V(lambda e: e.tensor_tensor(out=t1.ap, in0=t1.ap, in1=hngt.ap, op=ALU.mult), [t1, hngt], [t1])
                    V(lambda e, c=c, hd=hd: e.tensor_tensor(out=yst[:, c, hd * 128:(hd + 1) * 128], in0=t1.ap, in1=gtok[:, c, hd * 128:(hd + 1) * 128], op=ALU.mult),
                      [t1, gtok], [yst])
            P.final(dma(yhv[:, j * 8:(j + 1) * 8, :], yst.ap, reads=[yst]))
        for j in range(NJ):
            block_body(j)
        P.emit()
    print("k1 ops recorded", P.count)
    return nc


_OFF = np.cumsum((0,) + IN_SIZES)
_BF = ml_dtypes.bfloat16


def _consts_k1():
    ki = np.arange(128)[:, None, None] + 128 * np.arange(4)[None, :, None]
    qi = np.arange(512)[None, None, :]
    mF = np.where(ki <= qi, 0.0, -30000.0).astype(np.float32)
    mD = np.where(ki // 64 <= qi // 64, 0.0, -30000.0).astype(np.float32)
    mH = (np.arange(64)[:, None] <= np.arange(64)[None, :]).astype(np.float32)
    cst = np.zeros((44, 8), np.float32)
    sel = np.zeros((44, 4, 70), np.float32)
    for hd in range(2):
        b0 = 32 * hd
        for i in range(3):
            cst[b0 + i, i] = 1.0; cst[b0 + i, 3] = 8.0
            cst[b0 + 9 + i, i] = 1.0; cst[b0 + 9 + i, 3] = -8.0
            cst[b0 + 3 + i, 4] = 1.0; cst[b0 + 6 + i, 4] = 1.0
        for i in range(6):
            sel[b0 + i, 2 * hd, 64 + i] = 1.0
            sel[b0 + 6 + i, 2 * hd + 1, 64 + i] = 1.0
    return dict(maskF=mF, maskD=mD, maskH=mH, cst=cst, sel=sel, ident=np.eye(128, dtype=np.float32))


def prep_k1(p, layer, hh):
    w = p["w_in"][layer]
    o = _OFF
    m = dict(_consts_k1())
    pad70 = lambda a: np.concatenate([a, np.zeros((D, 6), np.float32)], axis=1)
    m["wfq"] = np.stack([pad70(w[:, o[0] + (2 * hh + l) * 64: o[0] + (2 * hh + l + 1) * 64]) for l in range(2)])
    m["wfk"] = np.stack([pad70(w[:, o[1] + (2 * hh + l) * 64: o[1] + (2 * hh + l + 1) * 64]) for l in range(2)])
    m["wfv"] = np.ascontiguousarray(w[:, o[2] + hh * 128: o[2] + (hh + 1) * 128])
    wff = np.zeros((D, 44), np.float32)
    fbr = np.zeros((44, 1), np.float32)
    for l in range(2):
        wff[:, 32 * l:32 * l + 12] = w[:, o[3] + 2 * hh + l][:, None]
        fbr[32 * l:32 * l + 12, 0] = p["fox_fb"][layer, 2 * hh + l]
    m["wff"] = wff; m["fbr"] = fbr
    m["wdq"] = np.stack([w[:, o[4] + (2 * hh + l) * 64: o[4] + (2 * hh + l + 1) * 64] for l in range(2)])
    m["wdk"] = np.stack([w[:, o[5] + (2 * hh + l) * 64: o[5] + (2 * hh + l + 1) * 64] for l in range(2)])
    m["wdv"] = np.ascontiguousarray(w[:, o[6] + hh * 128: o[6] + (hh + 1) * 128])
    for nm, i in (("whq", 7), ("whf", 8), ("whi", 9), ("whg", 10)):
        m[nm] = np.ascontiguousarray(w[:, o[i] + hh * 256: o[i] + (hh + 1) * 256])
    m["lamp"] = np.stack([p["lam_q1"][layer], p["lam_k1"][layer], p["lam_q2"][layer], p["lam_k2"][layer]])
    m["dng"] = np.ascontiguousarray(p["diff_norm_g"][layer][:, None])
    m["hlb"] = np.ascontiguousarray(p["hgrn_lb"][:, hh * 256:(hh + 1) * 256].reshape(2, 2, 128).transpose(2, 0, 1))
    m["hng"] = np.ascontiguousarray(p["hgrn_norm_g"][layer][None])
    return {k: np.ascontiguousarray(v, dtype=np.float32) for k, v in m.items()}


def prep_k2(p, layer):
    return dict(w_out=p["w_out"][layer], wq=p["mem_wq"][layer], wk=p["mem_wk"][layer], wv=p["mem_wv"][layer], wo=p["mem_wo"][layer],
                rw=p["router_w"], rb=p["router_b"][None], w1=p["w1"][layer], w3=p["w3"][layer], w2=p["w2"][layer],
                lng=p["ln_g"][layer], lnb=p["ln_b"][layer], ident=np.eye(128, dtype=np.float32))


_NC_CACHE = {}


def _get(name, fn, *a):
    key = (name,) + a
    if key not in _NC_CACHE:
        _NC_CACHE[key] = fn(*a)
    return _NC_CACHE[key]


def kernel(**inputs):
    p = {k: np.asarray(v) for k, v in inputs.items()}
    x = np.ascontiguousarray(p["x"], dtype=np.float32)
    mem = np.asarray(p["mem"], dtype=np.float32)
    ncore = 8
    HALF = SEQ // 2
    cores = list(range(ncore))
    ident = np.eye(128, dtype=np.float32)
    nc0 = _get("k0", build_k0, HALF)
    im = [dict(x=np.ascontiguousarray(x[c // 2, (c % 2) * HALF:(c % 2 + 1) * HALF]), g=p["ln_in_g"][None].astype(np.float32),
               b=p["ln_in_b"][None].astype(np.float32), ident=ident) for c in cores]
    r = run_bass_kernel_spmd(nc0, im, core_ids=cores).results
    h = [r[c]["h"] for c in cores]
    hT = [r[c]["hT"] for c in cores]
    memT = [np.ascontiguousarray(mem[b].T) for b in range(BATCH)]
    for layer in range(DEPTH):
        nc1 = _get("k1", build_k1, SEQ, layer)
        im = []
        for c in cores:
            b, hh = c // 2, c % 2
            m = prep_k1(p, layer, hh)
            m["hT"] = np.ascontiguousarray(np.concatenate([hT[2 * b], hT[2 * b + 1]], axis=1))
            im.append(m)
        r = run_bass_kernel_spmd(nc1, im, core_ids=cores).results
        nc2 = _get("k2", build_k2, HALF)
        w2m = {k: np.ascontiguousarray(v, dtype=np.float32) for k, v in prep_k2(p, layer).items()}
        im = []
        for c in cores:
            b, half = c // 2, c % 2
            sl = slice(half * HALF, (half + 1) * HALF)
            yT = np.concatenate([r[2 * b]["yf"][:, sl], r[2 * b + 1]["yf"][:, sl], r[2 * b]["yd"][:, sl], r[2 * b + 1]["yd"][:, sl],
                                 r[2 * b]["yh"][sl].T, r[2 * b + 1]["yh"][sl].T], axis=0)
            m = dict(w2m)
            m["h_in"] = h[c]; m["yT_in"] = np.ascontiguousarray(yT); m["memT"] = memT[b]
            im.append(m)
        r2 = run_bass_kernel_spmd(nc2, im, core_ids=cores).results
        h = [r2[c]["h_out"] for c in cores]
        hT = [r2[c]["hT_out"] for c in cores]
    out = np.stack([np.concatenate([h[2 * b], h[2 * b + 1]], axis=0) for b in range(BATCH)])
    return out.astype(np.float32)
```
